# Optimizing a Trainium2 kernel written in Bass

```python
import math
import jax
import jax.numpy as jnp
from jax import lax
import numpy as np

D_MODEL = 1024
BATCH = 2
SEQ = 8192
DEPTH = 2

N_MIXERS = 2
CONV_WIDTH = 3
N_HEADS = 8
HEAD_DIM = 64
V_DIM = 2 * HEAD_DIM
ROPE_THETA = 500000.0
ROT_DIM = HEAD_DIM // 4
Q_BLOCK = 128
D_FF = 2816
N_EXPERTS = 8
TOP_K = 2
D_FF_EXPERT = 3584
RMS_EPS = 1e-6
N_CONV_LAYERS = (DEPTH + 1) // 2
N_ATTN_LAYERS = DEPTH // 2

kernel_name = 'hybrid_shortconv_diffattn_moe_encoder'


def rmsnorm(x, g):
    xf = x.astype(jnp.float32)
    y = xf * lax.rsqrt(jnp.mean(xf * xf, axis=-1, keepdims=True) + RMS_EPS)
    return (y * g.astype(jnp.float32)).astype(x.dtype)


def swiglu(h, w1, w3, w2):
    return (jax.nn.silu(h @ w1) * (h @ w3)) @ w2


def short_conv_mixer(h, w_in, conv_w, w_out):
    S = h.shape[1]
    b_gate, c_gate, v = jnp.split(h @ w_in, 3, axis=-1)
    u = c_gate * v
    pad = CONV_WIDTH // 2
    up = jnp.pad(u, ((0, 0), (pad, pad), (0, 0)))
    conv = sum(up[:, j:j + S] * conv_w[j] for j in range(CONV_WIDTH))
    return (b_gate * conv) @ w_out


def rope_partial(t, cos, sin):
    half = ROT_DIM // 2
    t1 = t[..., :half]
    t2 = t[..., half:ROT_DIM]
    return jnp.concatenate([t1 * cos - t2 * sin, t2 * cos + t1 * sin, t[..., ROT_DIM:]], axis=-1)


def diff_attention(h, positions, w_in, q_norm, k_norm, lam_q1, lam_k1, lam_q2, lam_k2, subln, w_out, lambda_init):
    Bsz, S, _ = h.shape
    q, k, v = jnp.split(h @ w_in, 3, axis=-1)
    q = rmsnorm(q.reshape(Bsz, S, N_HEADS, 2, HEAD_DIM), q_norm)
    k = rmsnorm(k.reshape(Bsz, S, N_HEADS, 2, HEAD_DIM), k_norm)
    v = v.reshape(Bsz, S, N_HEADS, V_DIM)
    inv_freq = ROPE_THETA ** (-jnp.arange(0, ROT_DIM, 2, dtype=jnp.float32) / ROT_DIM)
    ang = positions.astype(jnp.float32)[..., None] * inv_freq
    cos = jnp.cos(ang)[:, :, None, None, :].astype(h.dtype)
    sin = jnp.sin(ang)[:, :, None, None, :].astype(h.dtype)
    q = rope_partial(q, cos, sin) * (HEAD_DIM ** -0.5)
    k = rope_partial(k, cos, sin)
    lam = (jnp.exp(jnp.sum(lam_q1.astype(jnp.float32) * lam_k1.astype(jnp.float32)))
           - jnp.exp(jnp.sum(lam_q2.astype(jnp.float32) * lam_k2.astype(jnp.float32)))
           + lambda_init)
    n_blk = S // Q_BLOCK
    qb = q.reshape(Bsz, n_blk, Q_BLOCK, N_HEADS, 2, HEAD_DIM).transpose(1, 0, 2, 3, 4, 5)

    def block(q_blk):
        s = jnp.einsum('bqhcd,bkhcd->bhcqk', q_blk, k).astype(jnp.float32)
        p = jax.nn.softmax(s, axis=-1)
        a = p[:, :, 0] - lam * p[:, :, 1]
        return jnp.einsum('bhqk,bkhe->bqhe', a.astype(v.dtype), v)

    o = lax.map(block, qb)
    o = o.transpose(1, 0, 2, 3, 4).reshape(Bsz, S, N_HEADS, V_DIM)
    o = rmsnorm(o, subln) * (1.0 - lambda_init)
    return o.reshape(Bsz, S, N_HEADS * V_DIM) @ w_out


def moe_swiglu(h, router, w1, w3, w2):
    Bsz, S, D = h.shape
    t = h.reshape(Bsz * S, D)
    logits = (t @ router).astype(jnp.float32)
    top_v, top_i = lax.top_k(logits, TOP_K)
    gates = jax.nn.softmax(top_v, axis=-1)
    combine = jnp.sum(jax.nn.one_hot(top_i, N_EXPERTS, dtype=jnp.float32) * gates[..., None], axis=1)
    out = jnp.zeros_like(t)
    for e in range(N_EXPERTS):
        out = out + combine[:, e:e + 1].astype(t.dtype) * swiglu(t, w1[e], w3[e], w2[e])
    return out.reshape(Bsz, S, D)


def setup_inputs(seed: int = 0) -> dict:
    key = jax.random.key(seed)
    ks = iter(jax.random.split(key, 32))
    D = D_MODEL
    QKV = 2 * N_HEADS * HEAD_DIM * 2 + N_HEADS * V_DIM

    def nrm(shape, scale):
        return jax.random.normal(next(ks), shape, jnp.float32) * scale

    def gain(shape):
        return 1.0 + 0.05 * jax.random.normal(next(ks), shape, jnp.float32)

    return {
        'x': jax.random.normal(next(ks), (BATCH, SEQ, D), jnp.float32),
        'positions': jnp.broadcast_to(jnp.arange(SEQ, dtype=jnp.int32), (BATCH, SEQ)),
        'norm_mix': gain((DEPTH, D)),
        'norm_ffn': gain((DEPTH, D)),
        'conv_in': nrm((N_CONV_LAYERS, D, 3 * D), D ** -0.5),
        'conv_w': nrm((N_CONV_LAYERS, CONV_WIDTH, D), CONV_WIDTH ** -0.5),
        'conv_out': nrm((N_CONV_LAYERS, D, D), D ** -0.5),
        'attn_in': nrm((N_ATTN_LAYERS, D, QKV), D ** -0.5),
        'q_norm': gain((N_ATTN_LAYERS, HEAD_DIM)),
        'k_norm': gain((N_ATTN_LAYERS, HEAD_DIM)),
        'lam_q1': nrm((N_ATTN_LAYERS, HEAD_DIM), 0.1),
        'lam_k1': nrm((N_ATTN_LAYERS, HEAD_DIM), 0.1),
        'lam_q2': nrm((N_ATTN_LAYERS, HEAD_DIM), 0.1),
        'lam_k2': nrm((N_ATTN_LAYERS, HEAD_DIM), 0.1),
        'subln': gain((N_ATTN_LAYERS, V_DIM)),
        'attn_out': nrm((N_ATTN_LAYERS, N_HEADS * V_DIM, D), (N_HEADS * V_DIM) ** -0.5),
        'ffn_w1': nrm((N_CONV_LAYERS, D, D_FF), D ** -0.5),
        'ffn_w3': nrm((N_CONV_LAYERS, D, D_FF), D ** -0.5),
        'ffn_w2': nrm((N_CONV_LAYERS, D_FF, D), D_FF ** -0.5),
        'router': nrm((N_ATTN_LAYERS, D, N_EXPERTS), D ** -0.5),
        'moe_w1': nrm((N_ATTN_LAYERS, N_EXPERTS, D, D_FF_EXPERT), D ** -0.5),
        'moe_w3': nrm((N_ATTN_LAYERS, N_EXPERTS, D, D_FF_EXPERT), D ** -0.5),
        'moe_w2': nrm((N_ATTN_LAYERS, N_EXPERTS, D_FF_EXPERT, D), D_FF_EXPERT ** -0.5),
    }


def reference(x, positions, norm_mix, norm_ffn, conv_in, conv_w, conv_out, attn_in, q_norm, k_norm,
              lam_q1, lam_k1, lam_q2, lam_k2, subln, attn_out, ffn_w1, ffn_w3, ffn_w2,
              router, moe_w1, moe_w3, moe_w2):
    for i in range(DEPTH):
        j = i // N_MIXERS
        h = rmsnorm(x, norm_mix[i])
        if i % N_MIXERS == 0:
            x = x + short_conv_mixer(h, conv_in[j], conv_w[j], conv_out[j])
        else:
            lambda_init = 0.8 - 0.6 * math.exp(-0.3 * i)
            x = x + diff_attention(h, positions, attn_in[j], q_norm[j], k_norm[j], lam_q1[j], lam_k1[j],
                                   lam_q2[j], lam_k2[j], subln[j], attn_out[j], lambda_init)
        h = rmsnorm(x, norm_ffn[i])
        f = i // 2
        if i % 2 == 0:
            x = x + swiglu(h, ffn_w1[f], ffn_w3[f], ffn_w2[f])
        else:
            x = x + moe_swiglu(h, router[f], moe_w1[f], moe_w3[f], moe_w2[f])
    return x
```

```python
import math
import numpy as np
import ml_dtypes
import concourse.bass as bass
import concourse.mybir as mybir
from concourse.bass_utils import run_bass_kernel_spmd

F32 = mybir.dt.float32
BF16 = mybir.dt.bfloat16
I32 = mybir.dt.int32
AF = mybir.ActivationFunctionType
ALU = mybir.AluOpType
AX = mybir.AxisListType

D = 1024
NT = 2048
SEQ = 8192
NH = 8
DFF = 2816
NE = 8
DFE = 3584
EPS = 1e-6
LAMBDA_INIT = 0.8 - 0.6 * math.exp(-0.3 * 1)
ROPE_THETA = 500000.0
TWO_PI = 2.0 * math.pi

ENG = ("pe", "act", "dve", "pool", "sp")
SAFE = False


class Res:
    __slots__ = ("name", "w", "r")

    def __init__(self, name):
        self.name = name
        self.w = None
        self.r = {}


class Op:
    __slots__ = ("eng", "fn", "deps", "idx", "signal", "sem", "val", "is_dma", "inc", "strict")


class KB:
    def __init__(self, nc):
        self.nc = nc
        self.ops = {e: [] for e in ENG}
        self.dma_sems = {}
        self.esem = {}

    def add(self, eng, fn, reads=(), writes=(), dma_key=None, inc=16, strict=False):
        op = Op()
        op.inc = inc
        op.strict = strict
        op.eng = eng
        op.fn = fn
        op.signal = False
        op.idx = 0
        op.is_dma = dma_key is not None
        op.sem = None
        op.val = 0
        deps = {}
        for r in reads:
            if r.w is not None:
                deps[id(r.w)] = r.w
        for w in writes:
            if w.w is not None:
                deps[id(w.w)] = w.w
            for d in w.r.values():
                deps[id(d)] = d
        if dma_key is not None:
            sem = self.dma_sems.get(dma_key)
            if sem is None:
                sem = [self.nc.alloc_semaphore("d_" + dma_key), 0]
                self.dma_sems[dma_key] = sem
            sem[1] += inc
            op.sem = sem
            op.val = sem[1]
            deps = {k: d for k, d in deps.items() if not (d.is_dma and d.sem is sem)}
        op.deps = list(deps.values())
        rk = dma_key if dma_key is not None else eng
        for r in reads:
            r.r[rk] = op
        for w in writes:
            w.w = op
            w.r = {}
        self.ops[eng].append(op)
        return op

    def emit(self):
        nc = self.nc
        for e in ENG:
            for op in self.ops[e]:
                for d in op.deps:
                    if not d.is_dma and (d.eng != op.eng or SAFE or op.strict):
                        d.signal = True
        for e in ENG:
            c = 0
            for op in self.ops[e]:
                if op.signal and not op.is_dma:
                    c += 1
                    op.idx = c
        for e in ENG:
            self.esem[e] = nc.alloc_semaphore("e_" + e)

        def run(ename, eng):
            known = {}
            for op in self.ops[ename]:
                for d in op.deps:
                    if d.is_dma:
                        sem, val, key = d.sem[0], d.val, id(d.sem)
                    else:
                        if d.eng == ename and not (SAFE or op.strict):
                            continue
                        sem, val, key = self.esem[d.eng], d.idx, d.eng
                    if known.get(key, 0) >= val:
                        continue
                    eng.wait_ge(sem, val)
                    known[key] = val
                ins = op.fn(eng)
                if ins is None:
                    continue
                if op.is_dma:
                    ins.then_inc(op.sem[0], op.inc)
                elif op.signal:
                    ins.then_inc(self.esem[ename], 1)

        with nc.Block() as block:
            @block.tensor
            def _(e):
                run("pe", e)

            @block.scalar
            def _(e):
                run("act", e)

            @block.vector
            def _(e):
                run("dve", e)

            @block.gpsimd
            def _(e):
                run("pool", e)

            @block.sync
            def _(e):
                run("sp", e)


def chunks(n, g):
    out = []
    s = 0
    while s < n:
        out.append((s, min(g, n - s)))
        s += g
    return out


def build_program(mode="fused", upto=99):
    nc = bass.Bass("TRN2", target_bir_lowering=False)
    kb = KB(nc)
    doA = mode in ("fused", "A")
    doB = mode in ("fused", "B")

    def din(name, shape, dt=F32):
        return nc.dram_tensor(name, list(shape), dt, kind="ExternalInput").ap()

    def dout(name, shape, dt=F32):
        return nc.dram_tensor(name, list(shape), dt, kind="ExternalOutput").ap()

    if doA:
        d_xT = din("xT", [128, 8, NT])
        d_xh = din("xh", [128, 8, 2])
        d_pos = din("pos", [128, NT], I32)
        d_conv_in = din("conv_in", [D, 3 * D])
        d_conv_out = din("conv_out", [D, D])
        d_convw = din("convw", [128, 8, 3])
        d_w13 = din("ffn_w13", [D, 2 * DFF])
        d_w2 = din("ffn_w2", [DFF, D])
        d_attn_in = din("attn_in", [D, 3 * D])
        d_rotm = din("rotm", [128, 128])
        d_invf = din("invf", [128, 1])
        d_qkg = din("qkg", [128, 2])
    d_gains = din("gains", [128, 4, 8])
    if doB:
        d_lamv = din("lamv", [128, 256])
        d_subln = din("subln", [128, 1])
        d_attn_out = din("attn_out", [D, D])
        d_router = din("router", [128, 8, NE])
        d_m13 = din("moe_w13", [NE, D, 2 * DFE])
        d_m2 = din("moe_w2", [NE, DFE, D])
        d_ident = din("ident", [128, 128])
        d_yT = dout("yT", [128, 8, NT])
        if upto < 99:
            d_dbgh = dout("dbg_h", [128, 8, NT + 2], BF16)
            d_dbgs = dout("dbg_s", [128, 37888], BF16)
    if mode == "A":
        d_xmid = dout("xmid", [128, 8, NT])
        d_qT = dout("qT", [128, 8, NT], BF16)
        d_kown = dout("k_own", [NH * 128, NT], BF16)
        d_vown = dout("v_own", [NH * 128, 16 * 128], BF16)
    elif mode == "B":
        d_xmid = din("xmid", [128, 8, NT])
        d_qT = din("qT", [128, 8, NT], BF16)
        d_kall = din("k_all", [4 * NH * 128, NT], BF16)
        d_vall = din("v_all", [4 * NH * 128, 16 * 128], BF16)
    else:
        d_kown = nc.dram_tensor("k_own", [NH * 128, NT], BF16).ap()
        d_vown = nc.dram_tensor("v_own", [NH * 128, 16 * 128], BF16).ap()
        d_kall = nc.dram_tensor("k_all", [4 * NH * 128, NT], BF16).ap()
        d_vall = nc.dram_tensor("v_all", [4 * NH * 128, 16 * 128], BF16).ap()

    XT = nc.alloc_sbuf_tensor("XT", [128, 8, NT], F32)
    HT = nc.alloc_sbuf_tensor("HT", [128, 8, NT + 2], BF16)
    WS = nc.alloc_sbuf_tensor("WS", [128, 4, 4096], BF16)
    SCRN = 37888
    SCR = nc.alloc_sbuf_tensor("SCR", [128, SCRN], BF16)
    gains = nc.alloc_sbuf_tensor("sb_gains", [128, 4, 8], F32)
    ones_bf = nc.alloc_sbuf_tensor("ones_bf", [128, 128], BF16)
    bd_bf = nc.alloc_sbuf_tensor("bd_bf", [128, 128], BF16)
    cst = nc.alloc_sbuf_tensor("sb_cst", [128, 8], F32)
    xh = nc.alloc_sbuf_tensor("sb_xh", [128, 8, 2], F32)
    small = nc.alloc_sbuf_tensor("sb_small", [128, 1024], F32)
    PS = [nc.alloc_psum_tensor(f"ps{i}", [128, 512], F32) for i in range(8)]

    class SAlloc:
        def __init__(self):
            self.off = 0

        def get(self, shape, dt):
            n = int(np.prod(shape))
            nb = n if dt == BF16 else 2 * n
            nb = (nb + 15) // 16 * 16
            assert self.off + nb <= SCRN, (self.off, nb, SCRN)
            ap = SCR[:, self.off:self.off + nb]
            self.off += nb
            if dt != BF16:
                ap = ap.bitcast(dt)
            if dt != BF16:
                ap = ap[:, 0:n]
            else:
                ap = ap[:, 0:n]
            if len(shape) == 2:
                return ap.rearrange("p (a b) -> p a b", b=shape[1])
            if len(shape) == 3:
                return ap.rearrange("p (a b c) -> p a b c", b=shape[1], c=shape[2])
            return ap

    R = lambda n: Res(n)
    r_ps = [R(f"ps{i}") for i in range(8)]
    r_xt = [R(f"xt{i}") for i in range(4)]
    r_ht = [R(f"ht{i}") for i in range(4)]
    r_hh = R("hthalo")
    r_ws = [R(f"ws{i}") for i in range(4)]
    r_cst = R("cst")
    r_gains = R("gains")
    r_xh = R("xh")

    ws_state = {"n": 0}

    def load_slab(src_ap, nk, ncol):
        i = ws_state["n"] % 4
        ws_state["n"] += 1
        assert nk * ncol <= 4096
        view = WS[:, i, 0:nk * ncol].rearrange("p (k f) -> p k f", f=ncol)
        src = src_ap.rearrange("(k p) f -> p k f", p=128)
        kb.add("pool", lambda e, view=view, src=src: e.dma_start(out=view, in_=src),
               writes=[r_ws[i]], dma_key=f"ws{i}")
        return view, r_ws[i]

    def mm_group(bank, pairs, reads, n=512, m=128):
        out = PS[bank][0:m, 0:n]

        def fn(e, out=out, pairs=pairs):
            ins = None
            last = len(pairs) - 1
            for i, (l, r) in enumerate(pairs):
                ins = e.matmul(out, lhsT=l, rhs=r, start=(i == 0), stop=(i == last))
            return ins
        return kb.add("pe", fn, reads=reads, writes=[r_ps[bank]])

    def c_init(e):
        e.memset(ones_bf[:], 1.0)
        e.memset(bd_bf[:], 0.0)
        e.memset(bd_bf[0:64, 0:64], 1.0)
        e.memset(bd_bf[64:128, 64:128], 1.0)
        e.memset(cst[:, 0:1], EPS)
        return e.memset(cst[:, 1:2], -math.pi)
    r_ones = R("ones")
    kb.add("dve", c_init, writes=[r_ones, r_cst])
    kb.add("sp", lambda e: e.dma_start(out=gains[:], in_=d_gains), writes=[r_gains], dma_key="m_gains")

    if doA:
        for c in range(8):
            kb.add("sp", lambda e, c=c: e.dma_start(out=XT[:, c, :], in_=d_xT[:, c, :]),
                   writes=r_xt, dma_key="xld")
        kb.add("sp", lambda e: e.dma_start(out=xh[:], in_=d_xh), writes=[r_xh], dma_key="m_xh")
    else:
        for c in range(8):
            kb.add("sp", lambda e, c=c: e.dma_start(out=XT[:, c, :], in_=d_xmid[:, c, :]),
                   writes=r_xt, dma_key="xld")

    def rmsnorm(gi, sa, h32=None, halo=False):
        sq = [sa.get([8, 512], BF16) for _ in range(2)]
        r_sq = [R("sq0"), R("sq1")]
        rstd = [sa.get([512], F32) for _ in range(2)]
        r_rstd = [R("rstd0"), R("rstd1")]
        blocks = [(XT[:, :, tb * 512:(tb + 1) * 512], HT[:, :, tb * 512:(tb + 1) * 512], 512, r_xt[tb], r_ht[tb], tb)
                  for tb in range(4)]
        if halo:
            blocks.append((xh[:], HT[:, :, NT:NT + 2], 2, r_xh, r_hh, None))
        for bi, (src, dst, n, rs, rd, tb) in enumerate(blocks):
            i = bi % 2
            bank = 6 + i
            kb.add("act", lambda e, i=i, src=src, n=n: e.activation(out=sq[i][:, :, 0:n], in_=src, func=AF.Square),
                   reads=[rs], writes=[r_sq[i]])
            mm_group(bank, [(ones_bf[:], sq[i][:, c, 0:n]) for c in range(8)], reads=[r_sq[i], r_ones], n=n)
            kb.add("act", lambda e, i=i, n=n, bank=bank: e.activation(
                out=rstd[i][:, 0:n], in_=PS[bank][:, 0:n], func=AF.Sqrt, bias=cst[:, 0:1], scale=1.0 / D),
                reads=[r_ps[bank], r_cst], writes=[r_rstd[i]])
            kb.add("dve", lambda e, i=i, n=n: e.reciprocal(out=rstd[i][:, 0:n], in_=rstd[i][:, 0:n]),
                   reads=[r_rstd[i]], writes=[r_rstd[i]], strict=(n < 128))
            for c in range(8):
                if h32 is not None and tb is not None:
                    hv = h32(tb)
                    kb.add("dve", lambda e, i=i, c=c, src=src, hv=hv: e.scalar_tensor_tensor(
                        out=hv[:, c, :], in0=src[:, c, :], scalar=gains[:, gi, c:c + 1], in1=rstd[i][:, :],
                        op0=ALU.mult, op1=ALU.mult), reads=[rs, r_rstd[i], r_gains], writes=[h32.res(tb)])
                    kb.add("act", lambda e, c=c, dst=dst, hv=hv: e.activation(out=dst[:, c, :], in_=hv[:, c, :], func=AF.Copy),
                           reads=[h32.res(tb)], writes=[rd])
                else:
                    kb.add("dve", lambda e, i=i, c=c, src=src, dst=dst, n=n: e.scalar_tensor_tensor(
                        out=dst[:, c, :], in0=src[:, c, :], scalar=gains[:, gi, c:c + 1], in1=rstd[i][:, 0:n],
                        op0=ALU.mult, op1=ALU.mult), reads=[rs, r_rstd[i], r_gains], writes=[rd], strict=(n < 128))

    def ffn_alloc(sa, G, with_comb):
        b = {"G": G}
        b["z"] = sa.get([G, NT], BF16)
        b["r_z"] = [R(f"z{t}") for t in range(4)]
        b["sab"] = [sa.get([512], BF16) for _ in range(3)]
        b["r_sab"] = [R(f"sa{i}") for i in range(3)]
        b["zt"] = [sa.get([512], BF16) for _ in range(2)] if with_comb else None
        b["r_zt"] = [R("zt0"), R("zt1")]
        b["cnt"] = {"ab": 0, "sa": 0, "o": 0, "zt": 0}
        return b

    def gated_ffn(w13_ap, w2_ap, nf, b, comb=None, r_comb=None):
        G, z, r_z, sab, r_sab, zt, r_zt, cnt = b["G"], b["z"], b["r_z"], b["sab"], b["r_sab"], b["zt"], b["r_zt"], b["cnt"]
        for (f0, g) in chunks(nf, G):
            slab = None
            for fl in range(g):
                f = f0 + fl
                if fl % 2 == 0:
                    nfs = min(2, g - fl)
                    slab, r_slab = load_slab(w13_ap[:, f * 256:(f + nfs) * 256], 8, nfs * 256)
                so = (fl % 2) * 256
                for tb in range(4):
                    pa = (cnt["ab"] % 2) * 2
                    cnt["ab"] += 1
                    rhs = [HT[:, kc, tb * 512:(tb + 1) * 512] for kc in range(8)]
                    mm_group(pa, [(slab[:, kc, so:so + 128], rhs[kc]) for kc in range(8)], reads=[r_slab, r_ht[tb]])
                    mm_group(pa + 1, [(slab[:, kc, so + 128:so + 256], rhs[kc]) for kc in range(8)], reads=[r_slab, r_ht[tb]])
                    si = cnt["sa"] % 3
                    cnt["sa"] += 1
                    kb.add("act", lambda e, si=si, pa=pa: e.activation(out=sab[si][:, :], in_=PS[pa][:, :], func=AF.Silu),
                           reads=[r_ps[pa]], writes=[r_sab[si]])
                    zdst = z[:, fl, tb * 512:(tb + 1) * 512]
                    if comb is None:
                        kb.add("dve", lambda e, si=si, pa=pa, zdst=zdst: e.tensor_tensor(
                            out=zdst, in0=sab[si][:, :], in1=PS[pa + 1][:, :], op=ALU.mult),
                            reads=[r_sab[si], r_ps[pa + 1]], writes=[r_z[tb]])
                    else:
                        zi = cnt["zt"] % 2
                        cnt["zt"] += 1
                        kb.add("dve", lambda e, si=si, pa=pa, zi=zi: e.tensor_tensor(
                            out=zt[zi][:, :], in0=sab[si][:, :], in1=PS[pa + 1][:, :], op=ALU.mult),
                            reads=[r_sab[si], r_ps[pa + 1]], writes=[r_zt[zi]])
                        kb.add("dve", lambda e, zi=zi, zdst=zdst, tb=tb: e.tensor_tensor(
                            out=zdst, in0=zt[zi][:, :], in1=comb[:, tb * 512:(tb + 1) * 512], op=ALU.mult),
                            reads=[r_zt[zi], r_comb], writes=[r_z[tb]])
            for half in range(2):
                slab2, r_slab2 = load_slab(w2_ap[f0 * 128:(f0 + g) * 128, half * 512:(half + 1) * 512], g, 512)
                for ol in range(4):
                    o = half * 4 + ol
                    for tb in range(4):
                        bank = 4 + cnt["o"] % 4
                        cnt["o"] += 1
                        mm_group(bank, [(slab2[:, k, ol * 128:(ol + 1) * 128], z[:, k, tb * 512:(tb + 1) * 512]) for k in range(g)],
                                 reads=[r_slab2, r_z[tb]])
                        xs = XT[:, o, tb * 512:(tb + 1) * 512]
                        kb.add("dve", lambda e, bank=bank, xs=xs: e.tensor_tensor(out=xs, in0=PS[bank][:, :], in1=xs, op=ALU.add),
                               reads=[r_ps[bank], r_xt[tb]], writes=[r_xt[tb]])

    r_bar = {e: R("bar_" + e) for e in ("pe", "act", "dve")}
    r_gate = {e: R("gate_" + e) for e in ("pe", "act", "dve")}

    def barrier(extra=()):
        extra = list(extra)
        kb.add("pe", lambda e: e.matmul(PS[0][:, 0:1], lhsT=ones_bf[:], rhs=ones_bf[:, 0:1], start=True, stop=True),
               reads=[r_ones] + extra, writes=[r_ps[0], r_bar["pe"]])
        kb.add("act", lambda e: e.activation(out=small[:, 0:1], in_=cst[:, 0:1], func=AF.Copy),
               reads=[r_cst] + extra, writes=[r_bar["act"]])
        kb.add("dve", lambda e: e.tensor_copy(out=small[:, 1:2], in_=cst[:, 0:1]),
               reads=[r_cst] + extra, writes=[r_bar["dve"]])
        allb = list(r_bar.values())
        kb.add("pe", lambda e: e.matmul(PS[0][:, 0:1], lhsT=ones_bf[:], rhs=ones_bf[:, 0:1], start=True, stop=True),
               reads=[r_ones] + allb, writes=[r_ps[0], r_gate["pe"]])
        kb.add("act", lambda e: e.activation(out=small[:, 2:3], in_=cst[:, 0:1], func=AF.Copy),
               reads=[r_cst] + allb, writes=[r_gate["act"]])
        kb.add("dve", lambda e: e.tensor_copy(out=small[:, 3:4], in_=cst[:, 0:1]),
               reads=[r_cst] + allb, writes=[r_gate["dve"]])

    def out_proj(w_ap, rhs_of, r_rhs_of, tbs, bank_mod=6):
        ocnt = 0
        for half in range(2):
            slab, r_slab = load_slab(w_ap[:, half * 512:(half + 1) * 512], 8, 512)
            for ol in range(4):
                o = half * 4 + ol
                for tb in tbs:
                    bank = ocnt % bank_mod
                    ocnt += 1
                    mm_group(bank, [(slab[:, kc, ol * 128:(ol + 1) * 128], rhs_of(kc, tb)) for kc in range(8)],
                             reads=[r_slab, r_rhs_of(tb)])
                    xs = XT[:, o, tb * 512:(tb + 1) * 512]
                    kb.add("dve", lambda e, bank=bank, xs=xs: e.tensor_tensor(out=xs, in0=PS[bank][:, :], in1=xs, op=ALU.add),
                           reads=[r_ps[bank], r_xt[tb]], writes=[r_xt[tb]])

    if doA:
        sa = SAlloc()
        rmsnorm(0, sa, halo=True)
        convw = sa.get([8, 3], F32)
        r_convw = R("convw")
        kb.add("sp", lambda e: e.dma_start(out=convw, in_=d_convw), reads=list(r_gate.values()), writes=[r_convw], dma_key="m_convw")
        gT = sa.get([8, 1024], BF16)
        r_gT = [R("gT0"), R("gT1")]
        hh = sa.get([8, 2], BF16)
        r_hhb = R("hhb")
        ub = [sa.get([1026], F32) for _ in range(2)]
        r_ub = [R("u0"), R("u1")]
        accb = [sa.get([1024], F32) for _ in range(2)]
        r_acc = [R("acc0"), R("acc1")]
        csb = [sa.get([512], F32) for _ in range(2)]
        r_csb = [R("csb0"), R("csb1")]
        bsb = [sa.get([1024], BF16) for _ in range(2)]
        r_bsb = [R("bsb0"), R("bsb1")]
        chalo = sa.get([2], F32)
        r_chalo = R("chalo")
        ccnt = 0
        for hf in range(2):
            t0 = hf * 1024
            Lc = NT if hf == 0 else 1023
            Rc = 1024 if hf == 0 else NT + 1
            r_L = r_hh if hf == 0 else r_ht[1]
            r_Rr = r_ht[2] if hf == 0 else r_hh

            def mkhh(e, Lc=Lc, Rc=Rc):
                e.tensor_copy(out=hh[:, :, 0:1], in_=HT[:, :, Lc:Lc + 1])
                return e.tensor_copy(out=hh[:, :, 1:2], in_=HT[:, :, Rc:Rc + 1])
            kb.add("dve", mkhh, reads=[r_L, r_Rr], writes=[r_hhb])
            for f in range(8):
                slab, r_slab = load_slab(d_conv_in[:, f * 384:(f + 1) * 384], 8, 384)
                ui = f % 2
                for tbl in range(2):
                    tb = hf * 2 + tbl
                    rhs = [HT[:, kc, tb * 512:(tb + 1) * 512] for kc in range(8)]
                    mm_group(tbl, [(slab[:, kc, 128:256], rhs[kc]) for kc in range(8)], reads=[r_slab, r_ht[tb]])
                    mm_group(2 + tbl, [(slab[:, kc, 256:384], rhs[kc]) for kc in range(8)], reads=[r_slab, r_ht[tb]])
                    ci = ccnt % 2
                    ccnt += 1
                    kb.add("act", lambda e, ci=ci, tbl=tbl: e.activation(out=csb[ci][:, :], in_=PS[tbl][:, :], func=AF.Copy),
                           reads=[r_ps[tbl]], writes=[r_csb[ci]])
                    kb.add("dve", lambda e, ci=ci, tbl=tbl, ui=ui: e.tensor_tensor(
                        out=ub[ui][:, 1 + tbl * 512:1 + (tbl + 1) * 512], in0=csb[ci][:, :], in1=PS[2 + tbl][:, :], op=ALU.mult),
                        reads=[r_csb[ci], r_ps[2 + tbl]], writes=[r_ub[ui]])
                mm_group(6, [(slab[:, kc, 128:256], hh[:, kc, :]) for kc in range(8)], reads=[r_slab, r_hhb], n=2)
                mm_group(7, [(slab[:, kc, 256:384], hh[:, kc, :]) for kc in range(8)], reads=[r_slab, r_hhb], n=2)
                kb.add("act", lambda e: e.activation(out=chalo[:, :], in_=PS[6][:, 0:2], func=AF.Copy),
                       reads=[r_ps[6]], writes=[r_chalo])

                def halo_u(e, ui=ui):
                    e.tensor_tensor(out=ub[ui][:, 0:1], in0=chalo[:, 0:1], in1=PS[7][:, 0:1], op=ALU.mult)
                    return e.tensor_tensor(out=ub[ui][:, 1025:1026], in0=chalo[:, 1:2], in1=PS[7][:, 1:2], op=ALU.mult)
                kb.add("dve", halo_u, reads=[r_chalo, r_ps[7]], writes=[r_ub[ui]])
                for tbl in range(2):
                    tb = hf * 2 + tbl
                    rhs = [HT[:, kc, tb * 512:(tb + 1) * 512] for kc in range(8)]
                    mm_group(4 + tbl, [(slab[:, kc, 0:128], rhs[kc]) for kc in range(8)], reads=[r_slab, r_ht[tb]])
                    kb.add("act", lambda e, ui=ui, tbl=tbl: e.activation(
                        out=bsb[ui][:, tbl * 512:(tbl + 1) * 512], in_=PS[4 + tbl][:, :], func=AF.Copy),
                        reads=[r_ps[4 + tbl]], writes=[r_bsb[ui]])

                def conv(e, ui=ui, f=f):
                    e.tensor_scalar(out=accb[ui][:, :], in0=ub[ui][:, 1:1025], scalar1=convw[:, f, 1:2], scalar2=None, op0=ALU.mult)
                    e.scalar_tensor_tensor(out=accb[ui][:, :], in0=ub[ui][:, 0:1024], scalar=convw[:, f, 0:1], in1=accb[ui][:, :],
                                           op0=ALU.mult, op1=ALU.add)
                    e.scalar_tensor_tensor(out=accb[ui][:, :], in0=ub[ui][:, 2:1026], scalar=convw[:, f, 2:3], in1=accb[ui][:, :],
                                           op0=ALU.mult, op1=ALU.add)
                    return e.tensor_tensor(out=gT[:, f, :], in0=accb[ui][:, :], in1=bsb[ui][:, :], op=ALU.mult)
                kb.add("dve", conv, reads=[r_ub[ui], r_bsb[ui], r_convw], writes=[r_acc[ui], r_gT[0], r_gT[1]])
            ocnt = 0
            for half in range(2):
                slab, r_slab = load_slab(d_conv_out[:, half * 512:(half + 1) * 512], 8, 512)
                for ol in range(4):
                    o = half * 4 + ol
                    for tbl in range(2):
                        tb = hf * 2 + tbl
                        bank = ocnt % 6
                        ocnt += 1
                        mm_group(bank, [(slab[:, kc, ol * 128:(ol + 1) * 128], gT[:, kc, tbl * 512:(tbl + 1) * 512]) for kc in range(8)],
                                 reads=[r_slab, r_gT[tbl]])
                        xs = XT[:, o, tb * 512:(tb + 1) * 512]
                        kb.add("dve", lambda e, bank=bank, xs=xs: e.tensor_tensor(out=xs, in0=PS[bank][:, :], in1=xs, op=ALU.add),
                               reads=[r_ps[bank], r_xt[tb]], writes=[r_xt[tb]])

        if upto >= 2:
            barrier()
            sa = SAlloc()
            rmsnorm(1, sa)
            gated_ffn(d_w13, d_w2, DFF // 128, ffn_alloc(sa, 6, False))

    r_qt = R("QT")
    r_kst = R("kst")
    r_vst = R("vst")
    r_kown = R("kown")
    r_vown = R("vown")
    r_kall = R("kall")
    r_vall = R("vall")

    if doA and upto >= 3:
        barrier()
        sa = SAlloc()
        QT = sa.get([8, NT], BF16)
        rmsnorm(2, sa)
        cosF = sa.get([NT], BF16)
        sinF = sa.get([NT], BF16)
        r_tab = R("tab")
        qraw = sa.get([512], F32)
        sqb = sa.get([512], BF16)
        rsb = sa.get([512], F32)
        qn = sa.get([512], BF16)
        t1 = sa.get([512], F32)
        kst = sa.get([NT], BF16)
        vst = sa.get([1024], BF16)
        r_qraw, r_sqb, r_rsb, r_qn, r_t1 = R("qraw"), R("sqb"), R("rsb"), R("qn"), R("t1")
        rotm = small[:, 128:256]
        rotb = small[:, 384:448].bitcast(BF16)
        invf = small[:, 16:17]
        qkg = small[:, 18:20]
        r_rot = R("rot")
        kb.add("sp", lambda e: e.dma_start(out=rotm, in_=d_rotm), writes=[r_rot], dma_key="m_rot")
        r_invf = R("invf")
        kb.add("sp", lambda e: e.dma_start(out=invf, in_=d_invf), writes=[r_invf], dma_key="m_invf")
        r_qkg = R("qkg")
        kb.add("sp", lambda e: e.dma_start(out=qkg, in_=d_qkg), writes=[r_qkg], dma_key="m_qkg")
        kb.add("dve", lambda e: e.tensor_copy(out=rotb[:, :], in_=rotm), reads=[r_rot], writes=[r_rot])
        bufA_i = WS[:, 3, :].bitcast(I32)
        bufA = WS[:, 3, :].bitcast(F32)
        bufB = WS[:, 2, :].bitcast(F32)
        kb.add("sp", lambda e: e.dma_start(out=bufA_i, in_=d_pos), writes=[r_ws[3]], dma_key="ws3")

        bufB_i = WS[:, 2, :].bitcast(I32)
        INV2PI = 1.0 / TWO_PI

        def tabs(e):
            e.tensor_copy(out=bufA, in_=bufA_i)
            e.tensor_scalar(out=bufA, in0=bufA, scalar1=invf, scalar2=None, op0=ALU.mult)
            e.tensor_scalar(out=bufB, in0=bufA, scalar1=INV2PI, scalar2=None, op0=ALU.mult)
            e.tensor_copy(out=bufB_i, in_=bufB)
            e.tensor_copy(out=bufB, in_=bufB_i)
            e.scalar_tensor_tensor(out=bufB, in0=bufB, scalar=-TWO_PI, in1=bufA, op0=ALU.mult, op1=ALU.add)
            return e.tensor_scalar(out=bufB, in0=bufB, scalar1=math.pi, scalar2=-math.pi, op0=ALU.min, op1=ALU.max)
        kb.add("dve", tabs, reads=[r_invf], writes=[r_ws[3], r_ws[2]])
        kb.add("act", lambda e: e.activation(out=sinF[:, :], in_=bufB, func=AF.Sin),
               reads=[r_ws[2]], writes=[r_tab])

        def tabc(e):
            e.tensor_scalar(out=bufB, in0=bufA, scalar1=INV2PI, scalar2=0.25, op0=ALU.mult, op1=ALU.add)
            e.tensor_copy(out=bufB_i, in_=bufB)
            e.tensor_copy(out=bufB, in_=bufB_i)
            e.scalar_tensor_tensor(out=bufB, in0=bufB, scalar=-TWO_PI, in1=bufA, op0=ALU.mult, op1=ALU.add)
            e.tensor_scalar(out=bufB, in0=bufB, scalar1=0.5 * math.pi, scalar2=math.pi, op0=ALU.add, op1=ALU.min)
            return e.tensor_scalar(out=bufB, in0=bufB, scalar1=-math.pi, scalar2=None, op0=ALU.max)
        kb.add("dve", tabc, reads=[r_ws[3]], writes=[r_ws[2]])
        kb.add("act", lambda e: e.activation(out=cosF[:, :], in_=bufB, func=AF.Sin),
               reads=[r_ws[2]], writes=[r_tab])
        bcnt = {"a": 0, "b": 0}
        for which in range(2):
            for hs in range(2):
                slab, r_slab = load_slab(d_attn_in[:, which * D + hs * 512: which * D + (hs + 1) * 512], 8, 512)
                for hl in range(4):
                    h = hs * 4 + hl
                    for tb in range(4):
                        bA = bcnt["a"] % 4
                        bcnt["a"] += 1
                        bB = 4 + bcnt["b"] % 2
                        bC = 6 + bcnt["b"] % 2
                        bcnt["b"] += 1
                        mm_group(bA, [(slab[:, kc, hl * 128:(hl + 1) * 128], HT[:, kc, tb * 512:(tb + 1) * 512]) for kc in range(8)],
                                 reads=[r_slab, r_ht[tb]])
                        kb.add("act", lambda e, bA=bA: e.activation(out=sqb[:, :], in_=PS[bA][:, :], func=AF.Square),
                               reads=[r_ps[bA]], writes=[r_sqb])
                        kb.add("act", lambda e, bA=bA: e.activation(out=qraw[:, :], in_=PS[bA][:, :], func=AF.Copy),
                               reads=[r_ps[bA]], writes=[r_qraw])
                        mm_group(bB, [(bd_bf[:], sqb[:, :])], reads=[r_sqb, r_ones])
                        kb.add("act", lambda e, bB=bB: e.activation(out=rsb[:, :], in_=PS[bB][:, :], func=AF.Sqrt,
                                                                    bias=cst[:, 0:1], scale=1.0 / 64),
                               reads=[r_ps[bB], r_cst], writes=[r_rsb])
                        kb.add("dve", lambda e: e.reciprocal(out=rsb[:, :], in_=rsb[:, :]), reads=[r_rsb], writes=[r_rsb])
                        kb.add("dve", lambda e, which=which: e.scalar_tensor_tensor(
                            out=qn[:, :], in0=qraw[:, :], scalar=qkg[:, which:which + 1], in1=rsb[:, :], op0=ALU.mult, op1=ALU.mult),
                            reads=[r_qraw, r_rsb, r_qkg], writes=[r_qn])
                        mm_group(bC, [(rotb[:, :], qn[:, :])], reads=[r_qn, r_rot])
                        csl = slice(tb * 512, (tb + 1) * 512)
                        dst = QT[:, h, csl] if which == 0 else kst[:, csl]
                        r_dst = r_qt if which == 0 else r_kst

                        def rope(e, bC=bC, csl=csl, dst=dst):
                            e.tensor_tensor(out=t1[:, :], in0=qn[:, :], in1=cosF[:, csl], op=ALU.mult)
                            e.tensor_tensor(out=qraw[:, :], in0=PS[bC][:, :], in1=sinF[:, csl], op=ALU.mult)
                            return e.tensor_tensor(out=dst, in0=t1[:, :], in1=qraw[:, :], op=ALU.add)
                        kb.add("dve", rope, reads=[r_qn, r_ps[bC], r_tab, r_qraw], writes=[r_t1, r_qraw, r_dst])
                    if which == 1:
                        kb.add("sp", lambda e, h=h: e.dma_start(out=d_kown[h * 128:(h + 1) * 128, :], in_=kst[:, :]),
                               reads=[r_kst], writes=[r_kown], dma_key="kst")
        vs0, r_vs0 = load_slab(d_attn_in[:, 2 * D:2 * D + 512], 8, 512)
        vs1, r_vs1 = load_slab(d_attn_in[:, 2 * D + 512:3 * D], 8, 512)
        vslabs = [(vs0, r_vs0), (vs1, r_vs1)]
        vdst = d_vown.rearrange("(h p) (t e) -> p h t e", p=128, e=128)
        for tt in range(16):
            tb = tt // 4
            for half in range(2):
                bank = (tt * 2 + half) % 4
                vs, r_vs = vslabs[half]
                mm_group(bank, [(HT[:, kc, tt * 128:(tt + 1) * 128], vs[:, kc, :]) for kc in range(8)], reads=[r_vs, r_ht[tb]])
                kb.add("act", lambda e, bank=bank, half=half: e.activation(
                    out=vst[:, half * 512:(half + 1) * 512], in_=PS[bank][:, :], func=AF.Copy),
                    reads=[r_ps[bank]], writes=[r_vst])
            kb.add("sp", lambda e, tt=tt: e.dma_start(out=vdst[:, :, tt, :], in_=vst[:, :].rearrange("p (h e) -> p h e", e=128)),
                   reads=[r_vst], writes=[r_vown], dma_key="vst")

    if mode == "A":
        r_out = R("out")
        for c in range(8):
            kb.add("sp", lambda e, c=c: e.dma_start(out=d_xmid[:, c, :], in_=XT[:, c, :]), reads=r_xt + [r_out], dma_key="out")
        if upto >= 3:
            for c in range(8):
                kb.add("sp", lambda e, c=c: e.dma_start(out=d_qT[:, c, :], in_=QT[:, c, :]), reads=[r_qt, r_out], dma_key="out")
        kb.add("sp", lambda e: None, reads=[r_kown, r_vown], writes=[r_out])
        kb.emit()
        return nc

    if mode == "fused":
        groups = [[0, 1, 2, 3], [4, 5, 6, 7]]
        kb.add("pool", lambda e: e.collective_compute("AllGather", ALU.bypass, replica_groups=groups,
                                                      ins=[d_kown], outs=[d_kall]),
               reads=[r_kown], writes=[r_kall], dma_key="cc_k", inc=1)
        kb.add("pool", lambda e: e.collective_compute("AllGather", ALU.bypass, replica_groups=groups,
                                                      ins=[d_vown], outs=[d_vall]),
               reads=[r_vown], writes=[r_vall], dma_key="cc_v", inc=1)

    barrier(extra=[r_kst, r_vst])
    sa = SAlloc()
    QT = sa.get([8, NT], BF16)
    if mode == "B":
        for c in range(8):
            kb.add("sp", lambda e, c=c: e.dma_start(out=QT[:, c, :], in_=d_qT[:, c, :]), reads=list(r_gate.values()), writes=[r_qt], dma_key="qld")
    E = [[sa.get([512], BF16) for _ in range(3)] for _ in range(2)]
    r_E = [[R(f"E{c}{i}") for i in range(3)] for c in range(2)]
    osb = [sa.get([512], F32) for _ in range(2)]
    r_osb = [R("o1s"), R("o2s")]
    rcp = [sa.get([512], F32) for _ in range(2)]
    r_rcp = [R("r1"), R("r2")]
    ob = sa.get([512], F32)
    r_ob = R("ob")
    sqo = sa.get([512], BF16)
    r_sqo = R("sqo")
    rso = sa.get([512], F32)
    r_rso = R("rso")
    lam_sb = sa.get([256], F32)
    lam_t = sa.get([4], F32)
    sub_sb = small[:, 20:21]
    r_lam = R("lam")
    kb.add("sp", lambda e: e.dma_start(out=lam_sb[:, :], in_=d_lamv), reads=list(r_gate.values()), writes=[r_lam], dma_key="m_lam")
    r_sub = R("sub")
    kb.add("sp", lambda e: e.dma_start(out=sub_sb, in_=d_subln), writes=[r_sub], dma_key="m_sub")

    S = dict(strict=True)
    r_lam2 = R("lam2")

    def lam1(e):
        e.tensor_tensor(out=lam_sb[:, 0:64], in0=lam_sb[:, 0:64], in1=lam_sb[:, 64:128], op=ALU.mult)
        return e.tensor_tensor(out=lam_sb[:, 128:192], in0=lam_sb[:, 128:192], in1=lam_sb[:, 192:256], op=ALU.mult)
    kb.add("dve", lam1, reads=[r_lam], writes=[r_lam], **S)

    def lam1b(e):
        e.tensor_reduce(out=lam_t[:, 0:1], in_=lam_sb[:, 0:64], axis=AX.X, op=ALU.add)
        return e.tensor_reduce(out=lam_t[:, 1:2], in_=lam_sb[:, 128:192], axis=AX.X, op=ALU.add)
    kb.add("dve", lam1b, reads=[r_lam], writes=[r_lam2], **S)
    kb.add("act", lambda e: e.activation(out=lam_t[:, 2:4], in_=lam_t[:, 0:2], func=AF.Exp), reads=[r_lam2], writes=[r_lam2])

    def lam2(e):
        e.scalar_tensor_tensor(out=cst[:, 2:3], in0=lam_t[:, 3:4], scalar=-LAMBDA_INIT, in1=lam_t[:, 2:3],
                               op0=ALU.add, op1=ALU.subtract)
        return e.tensor_scalar(out=cst[:, 3:4], in0=sub_sb, scalar1=1.0 - LAMBDA_INIT, scalar2=None, op0=ALU.mult)
    kb.add("dve", lam2, reads=[r_lam2, r_sub], writes=[r_cst])

    def mm_acc(bank, lhsT, rhs, start, stop, reads):
        out = PS[bank][:, :]
        return kb.add("pe", lambda e: e.matmul(out, lhsT=lhsT, rhs=rhs, start=start, stop=stop),
                      reads=reads, writes=[r_ps[bank]])

    pending = []
    ecnt = 0
    sbank = 0
    for h in range(NH):
        for qb in range(4):
            qsl = slice(qb * 512, (qb + 1) * 512)
            slots = []
            for r in range(4):
                i = ws_state["n"] % 4
                ws_state["n"] += 1
                kview = WS[:, i, 0:2048]
                vview = WS[:, i, 2048:4096].rearrange("p (t e) -> p t e", e=128)
                row0 = r * NH * 128 + h * 128
                kb.add("sp", lambda e, kview=kview, row0=row0: e.dma_start(out=kview, in_=d_kall[row0:row0 + 128, :]),
                       reads=[r_kall], writes=[r_ws[i]], dma_key=f"ws{i}")
                kb.add("sp", lambda e, i=i, row0=row0: e.dma_start(out=WS[:, i, 2048:4096], in_=d_vall[row0:row0 + 128, :]),
                       reads=[r_vall], writes=[r_ws[i]], dma_key=f"ws{i}")
                slots.append((kview, vview, r_ws[i]))

            def qk(kt):
                kview, vview, r_slot = slots[kt // 16]
                j = kt % 16
                b1 = (kt % 2) * 2
                mm_group(b1, [(kview[0:64, j * 128:(j + 1) * 128], QT[0:64, h, qsl])], reads=[r_slot, r_qt])
                mm_group(b1 + 1, [(kview[64:128, j * 128:(j + 1) * 128], QT[64:128, h, qsl])], reads=[r_slot, r_qt])

            qk(0)
            for kt in range(64):
                if kt + 1 < 64:
                    qk(kt + 1)
                kview, vview, r_slot = slots[kt // 16]
                j = kt % 16
                b1 = (kt % 2) * 2
                ei = ecnt % 3
                ecnt += 1
                for c in range(2):
                    kb.add("act", lambda e, c=c, ei=ei, b1=b1: e.activation(out=E[c][ei][:, :], in_=PS[b1 + c][:, :], func=AF.Exp, scale=0.125),
                           reads=[r_ps[b1 + c]], writes=[r_E[c][ei]])
                for c in range(2):
                    mm_acc(4 + c, vview[:, j, :], E[c][ei][:, :], kt == 0, kt == 63, [r_slot, r_E[c][ei]])
                    mm_acc(6 + c, ones_bf[:], E[c][ei][:, :], kt == 0, kt == 63, [r_ones, r_E[c][ei]])
                if kt == 3 and pending:
                    pending.pop(0)()
            for c in range(2):
                kb.add("act", lambda e, c=c: e.activation(out=osb[c][:, :], in_=PS[4 + c][:, :], func=AF.Copy),
                       reads=[r_ps[4 + c]], writes=[r_osb[c]])
                kb.add("dve", lambda e, c=c: e.reciprocal(out=rcp[c][:, :], in_=PS[6 + c][:, :]),
                       reads=[r_ps[6 + c]], writes=[r_rcp[c]])

            def comb_o(e):
                e.tensor_tensor(out=osb[0][:, :], in0=osb[0][:, :], in1=rcp[0][:, :], op=ALU.mult)
                e.tensor_tensor(out=osb[1][:, :], in0=osb[1][:, :], in1=rcp[1][:, :], op=ALU.mult)
                return e.scalar_tensor_tensor(out=ob[:, :], in0=osb[1][:, :], scalar=cst[:, 2:3], in1=osb[0][:, :],
                                              op0=ALU.mult, op1=ALU.add)
            kb.add("dve", comb_o, reads=[r_osb[0], r_osb[1], r_rcp[0], r_rcp[1], r_cst], writes=[r_osb[0], r_osb[1], r_ob])
            kb.add("act", lambda e: e.activation(out=sqo[:, :], in_=ob[:, :], func=AF.Square), reads=[r_ob], writes=[r_sqo])

            def epi2(h=h, qb=qb, qsl=qsl):
                bank = 0
                bank = 2
                mm_group(bank, [(ones_bf[:], sqo[:, :])], reads=[r_sqo, r_ones])
                kb.add("act", lambda e: e.activation(out=rso[:, :], in_=PS[bank][:, :], func=AF.Sqrt, bias=cst[:, 0:1], scale=1.0 / 128),
                       reads=[r_ps[bank], r_cst], writes=[r_rso])
                kb.add("dve", lambda e: e.reciprocal(out=rso[:, :], in_=rso[:, :]), reads=[r_rso], writes=[r_rso])
                kb.add("dve", lambda e: e.scalar_tensor_tensor(out=HT[:, h, qsl], in0=ob[:, :], scalar=cst[:, 3:4], in1=rso[:, :],
                                                               op0=ALU.mult, op1=ALU.mult),
                       reads=[r_ob, r_rso, r_cst], writes=[r_ht[qb]])
            pending.append(epi2)
    while pending:
        pending.pop(0)()

    def dbg_dump():
        r_out = R("out")
        allres = r_xt + r_ht + [r_cst]
        for c in range(8):
            kb.add("sp", lambda e, c=c: e.dma_start(out=d_yT[:, c, :], in_=XT[:, c, :]), reads=allres + [r_out], dma_key="out")
        kb.add("sp", lambda e: e.dma_start(out=d_dbgh, in_=HT[:]), reads=allres + [r_out], dma_key="out")
        kb.add("sp", lambda e: e.dma_start(out=d_dbgs, in_=SCR[:]), reads=allres + [r_out], dma_key="out")
        kb.add("sp", lambda e: None, writes=[r_out])
        kb.emit()
        return nc

    if upto == 4:
        barrier()
        return dbg_dump()
    out_proj(d_attn_out, lambda kc, tb: HT[:, kc, tb * 512:(tb + 1) * 512], lambda tb: r_ht[tb], range(4), bank_mod=8)

    if upto == 5:
        barrier()
        return dbg_dump()
    barrier()
    sa = SAlloc()
    logits = sa.get([16, NE], F32)
    r_logits = R("logits")
    eq1 = sa.get([16, NE], F32)
    eq2 = sa.get([16, NE], F32)
    lm = sa.get([16, NE], F32)
    combt = sa.get([16, NE], F32)
    m1 = sa.get([16], F32)
    m2 = sa.get([16], F32)
    g1 = sa.get([16], F32)
    g2 = sa.get([16], F32)
    r_gate_t = R("gates")
    onesf = sa.get([128], F32)
    comb_bc = [sa.get([NT], F32) for _ in range(2)]
    r_cbc = [R("cbc0"), R("cbc1")]
    cbm = [sa.get([128], F32) for _ in range(2)]
    r_cbm = [R("cbm0"), R("cbm1")]
    sa_mark = sa.off
    h32b = sa.get([8, 512], F32)
    r_h32 = R("h32")
    router_sb = sa.get([8, NE], F32)
    r_router = R("router")
    kb.add("sp", lambda e: e.dma_start(out=router_sb, in_=d_router), reads=list(r_gate.values()), writes=[r_router], dma_key="m_router")
    ident = small[:, 256:384]
    r_ident = R("ident")
    kb.add("sp", lambda e: e.dma_start(out=ident, in_=d_ident), writes=[r_ident], dma_key="m_ident")
    sq = [sa.get([8, 512], BF16) for _ in range(1)]
    rstd = sa.get([512], F32)
    r_sq, r_rstd = R("sq"), R("rstd")
    for tb in range(4):
        src = XT[:, :, tb * 512:(tb + 1) * 512]
        dst = HT[:, :, tb * 512:(tb + 1) * 512]
        kb.add("act", lambda e, src=src: e.activation(out=sq[0][:, :, :], in_=src, func=AF.Square), reads=[r_xt[tb]], writes=[r_sq])
        mm_group(6, [(ones_bf[:], sq[0][:, c, :]) for c in range(8)], reads=[r_sq, r_ones])
        kb.add("act", lambda e: e.activation(out=rstd[:, :], in_=PS[6][:, :], func=AF.Sqrt, bias=cst[:, 0:1], scale=1.0 / D),
               reads=[r_ps[6], r_cst], writes=[r_rstd])
        kb.add("dve", lambda e: e.reciprocal(out=rstd[:, :], in_=rstd[:, :]), reads=[r_rstd], writes=[r_rstd])

        def hcalc(e, src=src):
            ins = None
            for c in range(8):
                ins = e.scalar_tensor_tensor(out=h32b[:, c, :], in0=src[:, c, :], scalar=gains[:, 3, c:c + 1], in1=rstd[:, :],
                                             op0=ALU.mult, op1=ALU.mult)
            return ins
        kb.add("dve", hcalc, reads=[r_xt[tb], r_rstd, r_gains], writes=[r_h32])
        kb.add("act", lambda e, dst=dst: e.activation(out=dst, in_=h32b[:, :, :], func=AF.Copy), reads=[r_h32], writes=[r_ht[tb]])
        for tl in range(4):
            tt = tb * 4 + tl
            mm_group(7, [(h32b[:, kc, tl * 128:(tl + 1) * 128], router_sb[:, kc, :]) for kc in range(8)],
                     reads=[r_h32, r_router], n=NE)
            kb.add("act", lambda e, tt=tt: e.activation(out=logits[:, tt, :], in_=PS[7][:, 0:NE], func=AF.Copy),
                   reads=[r_ps[7]], writes=[r_logits])

    def bc(ap2):
        return ap2.unsqueeze(2).to_broadcast([128, 16, NE])
    r_m1, r_m2, r_eq1, r_eq2, r_lm, r_g1, r_g2 = R("m1"), R("m2"), R("eq1"), R("eq2"), R("lm"), R("g1"), R("g2")
    S = dict(strict=True)
    kb.add("dve", lambda e: e.tensor_reduce(out=m1[:, :], in_=logits[:, :, :], axis=AX.X, op=ALU.max),
           reads=[r_logits], writes=[r_m1], **S)
    kb.add("dve", lambda e: e.tensor_tensor(out=eq1[:, :, :], in0=logits[:, :, :], in1=bc(m1[:, :]), op=ALU.is_equal),
           reads=[r_logits, r_m1], writes=[r_eq1], **S)
    kb.add("dve", lambda e: e.scalar_tensor_tensor(out=lm[:, :, :], in0=eq1[:, :, :], scalar=-1.0e30, in1=logits[:, :, :],
                                                   op0=ALU.mult, op1=ALU.add), reads=[r_eq1, r_logits], writes=[r_lm], **S)
    kb.add("dve", lambda e: e.tensor_reduce(out=m2[:, :], in_=lm[:, :, :], axis=AX.X, op=ALU.max), reads=[r_lm], writes=[r_m2], **S)
    kb.add("dve", lambda e: e.tensor_tensor(out=eq2[:, :, :], in0=lm[:, :, :], in1=bc(m2[:, :]), op=ALU.is_equal),
           reads=[r_lm, r_m2], writes=[r_eq2], **S)
    kb.add("dve", lambda e: e.tensor_tensor(out=g2[:, :], in0=m2[:, :], in1=m1[:, :], op=ALU.subtract),
           reads=[r_m1, r_m2], writes=[r_g2], **S)
    kb.add("act", lambda e: e.activation(out=g2[:, :], in_=g2[:, :], func=AF.Sigmoid), reads=[r_g2], writes=[r_g2])
    kb.add("dve", lambda e: e.tensor_scalar(out=g1[:, :], in0=g2[:, :], scalar1=-1.0, scalar2=1.0, op0=ALU.mult, op1=ALU.add),
           reads=[r_g2], writes=[r_g1], **S)
    kb.add("dve", lambda e: e.tensor_tensor(out=combt[:, :, :], in0=eq1[:, :, :], in1=bc(g1[:, :]), op=ALU.mult),
           reads=[r_eq1, r_g1], writes=[r_gate_t], **S)
    kb.add("dve", lambda e: e.tensor_tensor(out=eq2[:, :, :], in0=eq2[:, :, :], in1=bc(g2[:, :]), op=ALU.mult),
           reads=[r_eq2, r_g2], writes=[r_eq2], **S)
    kb.add("dve", lambda e: e.tensor_tensor(out=combt[:, :, :], in0=combt[:, :, :], in1=eq2[:, :, :], op=ALU.add),
           reads=[r_eq2, r_gate_t], writes=[r_gate_t], **S)
    kb.add("dve", lambda e: e.memset(onesf[:, :], 1.0), reads=[r_gate_t], writes=[r_gate_t], **S)

    barrier()
    if upto == 6:
        return dbg_dump()
    sa.off = sa_mark
    fb = ffn_alloc(sa, 7, True)
    for ex in range(NE):
        ci = ex % 2
        for tb in range(4):
            for tl in range(4):
                tt = tb * 4 + tl
                bi = tt % 2
                kb.add("dve", lambda e, bi=bi, tt=tt, ex=ex: e.tensor_scalar(
                    out=cbm[bi][:, :], in0=onesf[:, :], scalar1=combt[:, tt, ex:ex + 1], scalar2=None, op0=ALU.mult),
                    reads=[r_gate_t], writes=[r_cbm[bi]])
                kb.add("pe", lambda e, bi=bi, tl=tl: e.matmul(PS[7][:, tl * 128:(tl + 1) * 128], lhsT=cbm[bi][:, :], rhs=ident,
                                                             start=True, stop=True),
                       reads=[r_cbm[bi], r_ident], writes=[r_ps[7]])
            kb.add("act", lambda e, ci=ci, tb=tb: e.activation(out=comb_bc[ci][:, tb * 512:(tb + 1) * 512], in_=PS[7][:, :], func=AF.Copy),
                   reads=[r_ps[7]], writes=[r_cbc[ci]])
        gated_ffn(d_m13[ex], d_m2[ex], DFE // 128, fb, comb=comb_bc[ci], r_comb=r_cbc[ci])

    r_out = R("out")
    for c in range(8):
        kb.add("sp", lambda e, c=c: e.dma_start(out=d_yT[:, c, :], in_=XT[:, c, :]), reads=r_xt + [r_out], dma_key="out")
    kb.add("sp", lambda e: None, writes=[r_out])
    kb.emit()
    return nc


def _prep_common(inp):
    f32 = np.float32
    out = {}
    g = np.stack([inp["norm_mix"][0], inp["norm_ffn"][0], inp["norm_mix"][1], inp["norm_ffn"][1]], 0)
    out["gains"] = np.ascontiguousarray(g.reshape(4, 8, 128).transpose(2, 0, 1)).astype(f32)
    ci = np.asarray(inp["conv_in"][0])
    out["conv_in"] = np.ascontiguousarray(ci.reshape(D, 3, 8, 128).transpose(0, 2, 1, 3).reshape(D, 3 * D))
    out["conv_out"] = np.ascontiguousarray(inp["conv_out"][0])
    out["convw"] = np.ascontiguousarray(np.asarray(inp["conv_w"][0]).reshape(3, 8, 128).transpose(2, 1, 0))
    w1 = np.asarray(inp["ffn_w1"][0]).reshape(D, DFF // 128, 1, 128)
    w3 = np.asarray(inp["ffn_w3"][0]).reshape(D, DFF // 128, 1, 128)
    out["ffn_w13"] = np.ascontiguousarray(np.concatenate([w1, w3], axis=2).reshape(D, 2 * DFF))
    out["ffn_w2"] = np.ascontiguousarray(inp["ffn_w2"][0])
    out["attn_in"] = np.ascontiguousarray(inp["attn_in"][0])
    return out


def _prep_A(inp):
    out = _prep_common(inp)
    rot = np.zeros((128, 128), np.float32)
    invf = np.zeros((128, 1), np.float32)
    inv_freq = (np.float32(ROPE_THETA) ** (-np.arange(0, 16, 2, dtype=np.float32) / np.float32(16))).astype(np.float32)
    for cb in (0, 64):
        for i in range(8):
            rot[cb + i + 8, cb + i] = -1.0
            rot[cb + i, cb + 8 + i] = 1.0
            invf[cb + i, 0] = inv_freq[i]
            invf[cb + 8 + i, 0] = inv_freq[i]
    out["rotm"] = rot
    out["invf"] = invf
    out["qkg"] = np.ascontiguousarray(np.stack([np.tile(inp["q_norm"][0], 2), np.tile(inp["k_norm"][0], 2)], 1)).astype(np.float32)
    return out


def _prep_B(inp):
    out = {}
    lam = np.concatenate([inp["lam_q1"][0], inp["lam_k1"][0], inp["lam_q2"][0], inp["lam_k2"][0]])[None, :]
    out["lamv"] = np.ascontiguousarray(np.broadcast_to(lam, (128, 256))).astype(np.float32)
    out["subln"] = np.ascontiguousarray(inp["subln"][0].reshape(128, 1)).astype(np.float32)
    out["attn_out"] = np.ascontiguousarray(inp["attn_out"][0])
    out["router"] = np.ascontiguousarray(np.asarray(inp["router"][0]).reshape(8, 128, NE).transpose(1, 0, 2))
    w1 = np.asarray(inp["moe_w1"][0]).reshape(NE, D, DFE // 128, 1, 128)
    w3 = np.asarray(inp["moe_w3"][0]).reshape(NE, D, DFE // 128, 1, 128)
    out["moe_w13"] = np.concatenate([w1, w3], axis=3).reshape(NE, D, 2 * DFE)
    out["moe_w2"] = np.ascontiguousarray(inp["moe_w2"][0])
    out["ident"] = np.eye(128, dtype=np.float32)
    return out


def _gains(inp):
    g = np.stack([inp["norm_mix"][0], inp["norm_ffn"][0], inp["norm_mix"][1], inp["norm_ffn"][1]], 0)
    return np.ascontiguousarray(g.reshape(4, 8, 128).transpose(2, 0, 1)).astype(np.float32)


def _per_core_x(x, c):
    b, r = c // 4, c % 4
    s0 = r * NT
    xs = x[b, s0:s0 + NT]
    xT = np.ascontiguousarray(xs.reshape(NT, 8, 128).transpose(2, 1, 0))
    halo = np.zeros((2, D), np.float32)
    if s0 > 0:
        halo[0] = x[b, s0 - 1]
    if s0 + NT < SEQ:
        halo[1] = x[b, s0 + NT]
    xh = np.ascontiguousarray(halo.reshape(2, 8, 128).transpose(2, 1, 0))
    return xT, xh


FUSED = False
_CACHE = {}


def _prog(mode):
    if mode not in _CACHE:
        _CACHE[mode] = build_program(mode)
    return _CACHE[mode]


def kernel(**inputs):
    inp = {k: np.asarray(v) for k, v in inputs.items()}
    x = inp["x"].astype(np.float32)
    pos = inp["positions"].astype(np.int32)
    cA = _prep_A(inp)
    cB = _prep_B(inp)
    cores = list(range(8))
    per = []
    for c in cores:
        b, r = c // 4, c % 4
        xT, xh = _per_core_x(x, c)
        p = np.ascontiguousarray(np.broadcast_to(pos[b, r * NT:(r + 1) * NT][None, :], (128, NT)))
        per.append((xT, xh, p))
    if FUSED:
        maps = []
        for c in cores:
            m = dict(cA)
            m.update(cB)
            m["xT"], m["xh"], m["pos"] = per[c]
            maps.append(m)
        res = run_bass_kernel_spmd(_prog("fused"), maps, core_ids=cores)
        outs = [r_["yT"] for r_ in res.results]
    else:
        maps = []
        for c in cores:
            m = dict(cA)
            m["xT"], m["xh"], m["pos"] = per[c]
            maps.append(m)
        resA = run_bass_kernel_spmd(_prog("A"), maps, core_ids=cores).results
        maps = []
        for c in cores:
            b = c // 4
            m = dict(cB)
            m["gains"] = cA["gains"]
            m["xmid"] = resA[c]["xmid"]
            m["qT"] = resA[c]["qT"]
            m["k_all"] = np.concatenate([resA[b * 4 + r]["k_own"] for r in range(4)], axis=0)
            m["v_all"] = np.concatenate([resA[b * 4 + r]["v_own"] for r in range(4)], axis=0)
            maps.append(m)
        resB = run_bass_kernel_spmd(_prog("B"), maps, core_ids=cores).results
        outs = [r_["yT"] for r_ in resB]
    y = np.empty((2, SEQ, D), np.float32)
    for c in cores:
        b, r = c // 4, c % 4
        y[b, r * NT:(r + 1) * NT] = outs[c].transpose(2, 1, 0).reshape(NT, D)
    return y
```
